# Optimizing a Trainium2 kernel written in Bass

```python
import math
import jax
import jax.numpy as jnp
from jax import lax
import numpy as np

D_MODEL = 1024
BATCH = 8
SEQ = 2048
DEPTH = 4

GRID_W = 64
CTX_LEN = 256
EPS = 1e-6
N_MOD = 6

ATT_HQ = 8
ATT_HKV = 2
ATT_GROUP = ATT_HQ // ATT_HKV
ATT_DH = 64
Q_BLOCK = 128
ROPE_THETA = 10000.0
ROPE_AXIS_PAIRS = ATT_DH // 4

DN_H = 4
DN_DK = 128
DN_DV = 128
DN_CONV = 4
DN_CHUNK = 64

CONF_CH = 512
CONF_KW = 31

N_EXPERTS = 32
TOP_K = 4
D_FF = 1024
SWIGLU_LIMIT = 7.0
SWIGLU_ALPHA = 1.702

N_BRANCH = 3
IN_SPLITS = (ATT_HQ * ATT_DH, ATT_HKV * ATT_DH, ATT_HKV * ATT_DH,
             DN_H * DN_DK, DN_H * DN_DK, DN_H * DN_DV, 2 * DN_H, 2 * DN_H, DN_H * DN_DV,
             2 * CONF_CH, N_BRANCH * D_MODEL)
P_IN = sum(IN_SPLITS)

kernel_name = 'hybrid_gqa_gdn_conformer_moe_dit'


def rms_norm(x, g):
    xf = x.astype(jnp.float32)
    y = xf * lax.rsqrt(jnp.mean(xf * xf, axis=-1, keepdims=True) + EPS)
    return (y * g.astype(jnp.float32)).astype(x.dtype)


def layer_norm(x, g, b):
    xf = x.astype(jnp.float32)
    mu = jnp.mean(xf, axis=-1, keepdims=True)
    var = jnp.mean(jnp.square(xf - mu), axis=-1, keepdims=True)
    y = (xf - mu) * lax.rsqrt(var + EPS) * g.astype(jnp.float32) + b.astype(jnp.float32)
    return y.astype(x.dtype)


def l2_norm(x):
    xf = x.astype(jnp.float32)
    return xf * lax.rsqrt(jnp.sum(xf * xf, axis=-1, keepdims=True) + EPS)


def modulate(h, shift, scale):
    return h * (1.0 + scale) + shift


def split_in(z):
    idx = []
    acc = 0
    for w in IN_SPLITS[:-1]:
        acc += w
        idx.append(acc)
    return jnp.split(z, idx, axis=-1)


def depthwise_conv(x, w):
    k = w.shape[0]
    return lax.conv_general_dilated(
        x, w[:, None, :], window_strides=(1,), padding=[((k - 1) // 2, k // 2)],
        dimension_numbers=('NWC', 'WIO', 'NWC'), feature_group_count=x.shape[-1])


def axial_rope_tables(rows):
    r, col = jnp.meshgrid(jnp.arange(rows, dtype=jnp.float32),
                          jnp.arange(GRID_W, dtype=jnp.float32), indexing='ij')
    inv = ROPE_THETA ** (-jnp.arange(ROPE_AXIS_PAIRS, dtype=jnp.float32) / ROPE_AXIS_PAIRS)
    ang = jnp.stack([r.reshape(-1)[:, None] * inv, col.reshape(-1)[:, None] * inv], axis=1)
    return jnp.cos(ang), jnp.sin(ang)


def apply_rope(x, cos, sin):
    shp = x.shape
    xr = x.reshape(shp[:3] + (2, 2, ROPE_AXIS_PAIRS)).astype(jnp.float32)
    x1, x2 = xr[..., 0, :], xr[..., 1, :]
    cs, sn = cos[None, :, None], sin[None, :, None]
    out = jnp.stack([x1 * cs - x2 * sn, x1 * sn + x2 * cs], axis=-2)
    return out.reshape(shp).astype(x.dtype)


def att_qkv(parts, qg, kg):
    lead = parts[0].shape[:-1]
    q = rms_norm(parts[0].reshape(lead + (ATT_HQ, ATT_DH)), qg)
    k = rms_norm(parts[1].reshape(lead + (ATT_HKV, ATT_DH)), kg)
    v = parts[2].reshape(lead + (ATT_HKV, ATT_DH))
    return q, k, v


def gqa_block(q, k, v):
    s = jnp.einsum('bqkgd,btkd->bkgqt', q, k).astype(jnp.float32) * (ATT_DH ** -0.5)
    p = jax.nn.softmax(s, axis=-1).astype(v.dtype)
    return jnp.einsum('bkgqt,btkd->bqkgd', p, v)


def attend_latent(q, k, v):
    b, s = q.shape[:2]
    nb = s // Q_BLOCK
    qb = q.reshape(b, nb, Q_BLOCK, ATT_HKV, ATT_GROUP, ATT_DH).swapaxes(0, 1)
    o = lax.map(lambda qq: gqa_block(qq, k, v), qb)
    return o.swapaxes(0, 1).reshape(b, s, ATT_HQ * ATT_DH)


def gated_delta_chunked(q, k, v, g, beta, state0):
    b, t, h, dk = q.shape
    n = t // DN_CHUNK
    f32 = jnp.float32

    def chunks(a):
        a = a.astype(f32).reshape((b, n, DN_CHUNK, h) + a.shape[3:])
        return jnp.moveaxis(a, (1, 3), (0, 2))

    qc = chunks(q) * (dk ** -0.5)
    kc, vc, gc, bc = chunks(k), chunks(v), chunks(g), chunks(beta)
    gcum = jnp.cumsum(gc, axis=-1)
    lower = jnp.tril(jnp.ones((DN_CHUNK, DN_CHUNK), dtype=bool))
    strict = jnp.tril(jnp.ones((DN_CHUNK, DN_CHUNK), dtype=bool), -1)
    decay = jnp.exp(jnp.where(lower, gcum[..., :, None] - gcum[..., None, :], -jnp.inf))
    kb = kc * bc[..., None]
    vb = vc * bc[..., None]
    lmat = jnp.where(strict, jnp.einsum('nbhid,nbhjd->nbhij', kb, kc) * decay, 0.0)
    eye = jnp.eye(DN_CHUNK, dtype=f32)
    tmat = lax.linalg.triangular_solve(lmat + eye, jnp.broadcast_to(eye, lmat.shape),
                                       left_side=True, lower=True, unit_diagonal=True)
    u = tmat @ vb
    w = tmat @ (kb * jnp.exp(gcum)[..., None])
    intra = jnp.where(lower, jnp.einsum('nbhid,nbhjd->nbhij', qc, kc) * decay, 0.0)

    def step(s_prev, xs):
        q_i, k_i, u_i, w_i, g_i, a_i = xs
        v_new = u_i - w_i @ s_prev
        o = (q_i * jnp.exp(g_i)[..., None]) @ s_prev + a_i @ v_new
        g_last = g_i[..., -1]
        k_dec = k_i * jnp.exp(g_last[..., None] - g_i)[..., None]
        s_new = s_prev * jnp.exp(g_last)[..., None, None] + jnp.einsum('bhcd,bhce->bhde', k_dec, v_new)
        return s_new, o

    s_fin, o = lax.scan(step, state0.astype(f32), (qc, kc, u, w, gcum, intra))
    o = jnp.moveaxis(o, (0, 2), (1, 3)).reshape(b, t, h, v.shape[-1])
    return o, s_fin


def dn_stream(parts, conv_w, a_log, dt_bias):
    qkv = jax.nn.silu(depthwise_conv(jnp.concatenate([parts[3], parts[4], parts[5]], axis=-1), conv_w))
    q, k, v = jnp.split(qkv, [DN_H * DN_DK, 2 * DN_H * DN_DK], axis=-1)
    lead = q.shape[:-1]
    q = l2_norm(q.reshape(lead + (DN_H, DN_DK)))
    k = l2_norm(k.reshape(lead + (DN_H, DN_DK)))
    v = v.reshape(lead + (DN_H, DN_DV))
    gshape = lead + (2, DN_H)
    beta = jax.nn.sigmoid(parts[6].reshape(gshape).astype(jnp.float32))
    g = -jnp.exp(a_log.astype(jnp.float32)) * jax.nn.softplus(
        parts[7].reshape(gshape).astype(jnp.float32) + dt_bias.astype(jnp.float32))
    return q, k, v, g, beta


def dn_bidir(q, k, v, g, beta, s_fwd, s_bwd):
    o_f, s_f = gated_delta_chunked(q, k, v, g[:, :, 0], beta[:, :, 0], s_fwd)
    rev = lambda a: jnp.flip(a, axis=1)
    o_b, s_b = gated_delta_chunked(rev(q), rev(k), rev(v), rev(g[:, :, 1]), rev(beta[:, :, 1]), s_bwd)
    return o_f + rev(o_b), s_f, s_b


def dn_out(o, z_gate, norm_g):
    o = o * lax.rsqrt(jnp.mean(o * o, axis=-1, keepdims=True) + EPS) * norm_g.astype(jnp.float32)
    o = o.reshape(z_gate.shape) * jax.nn.silu(z_gate.astype(jnp.float32))
    return o.astype(z_gate.dtype)


def conformer(z, dw_w, dw_b, ln_g, ln_b, w_pw):
    a, b = jnp.split(z, 2, axis=-1)
    u = depthwise_conv(a * jax.nn.sigmoid(b), dw_w) + dw_b
    u = jax.nn.silu(layer_norm(u, ln_g, ln_b))
    return u @ w_pw


def merge(y_att, y_dn, y_conf, z_gates, w_o):
    g = jax.nn.sigmoid(z_gates.reshape(z_gates.shape[:-1] + (N_BRANCH, D_MODEL)))
    y = g[..., 0, :] * y_att + g[..., 1, :] * y_dn + g[..., 2, :] * y_conf
    return y @ w_o


def moe(h, w_r, b_r, w_gu, b_gu, w_dn, b_dn):
    logits = (h @ w_r + b_r).astype(jnp.float32)
    top_v, top_i = lax.top_k(logits, TOP_K)
    top_w = jax.nn.softmax(top_v, axis=-1)
    combine = jnp.sum(jax.nn.one_hot(top_i, N_EXPERTS, dtype=jnp.float32) * top_w[..., None], axis=-2)
    out = jnp.zeros(h.shape, jnp.float32)
    for e in range(N_EXPERTS):
        gate, up = jnp.split(h @ w_gu[e] + b_gu[e], 2, axis=-1)
        gate = jnp.minimum(gate, SWIGLU_LIMIT)
        up = jnp.clip(up, -SWIGLU_LIMIT, SWIGLU_LIMIT)
        act = (up + 1.0) * (gate * jax.nn.sigmoid(SWIGLU_ALPHA * gate))
        out = out + combine[:, e:e + 1] * (act @ w_dn[e] + b_dn[e]).astype(jnp.float32)
    return out.astype(h.dtype)


def setup_inputs(seed: int = 0) -> dict:
    key = jax.random.key(seed)
    ks = jax.random.split(key, 32)
    L, D, f32 = DEPTH, D_MODEL, jnp.float32

    def nrm(i, shape, scale):
        return scale * jax.random.normal(ks[i], shape, f32)

    dt = jnp.exp(jax.random.uniform(ks[10], (L, 2, DN_H), f32, minval=math.log(1e-3), maxval=math.log(1e-1)))
    return {
        'x': nrm(0, (BATCH, SEQ, D), 1.0),
        'c': nrm(1, (BATCH, D), 1.0),
        'ctx': nrm(2, (BATCH, CTX_LEN, D), 1.0),
        'c_ctx': nrm(3, (D,), 1.0),
        'w_mod': nrm(4, (L, D, N_MOD * D), 0.5 * D ** -0.5),
        'b_mod': nrm(5, (L, N_MOD * D), 0.02),
        'norm1_g': 1.0 + nrm(6, (L, D), 0.02),
        'w_in': nrm(7, (L, D, P_IN), D ** -0.5),
        'q_norm_g': 1.0 + nrm(8, (L, ATT_DH), 0.02),
        'k_norm_g': 1.0 + nrm(9, (L, ATT_DH), 0.02),
        'dn_conv_w': nrm(11, (L, DN_CONV, 2 * DN_H * DN_DK + DN_H * DN_DV), DN_CONV ** -0.5),
        'dn_a_log': jnp.log(jax.random.uniform(ks[12], (L, 2, DN_H), f32, minval=1.0, maxval=16.0)),
        'dn_dt_bias': dt + jnp.log(-jnp.expm1(-dt)),
        'dn_norm_g': 1.0 + nrm(13, (L, DN_DV), 0.02),
        'conf_dw_w': nrm(14, (L, CONF_KW, CONF_CH), CONF_KW ** -0.5),
        'conf_dw_b': nrm(15, (L, CONF_CH), 0.02),
        'conf_ln_g': 1.0 + nrm(16, (L, CONF_CH), 0.02),
        'conf_ln_b': nrm(17, (L, CONF_CH), 0.02),
        'w_att_o': nrm(18, (L, ATT_HQ * ATT_DH, D), (ATT_HQ * ATT_DH) ** -0.5),
        'w_dn_o': nrm(19, (L, DN_H * DN_DV, D), (DN_H * DN_DV) ** -0.5),
        'w_conf_o': nrm(20, (L, CONF_CH, D), CONF_CH ** -0.5),
        'w_out': nrm(21, (L, D, D), D ** -0.5),
        'norm2_g': 1.0 + nrm(22, (L, D), 0.02),
        'w_router': nrm(23, (L, D, N_EXPERTS), D ** -0.5),
        'b_router': nrm(24, (L, N_EXPERTS), 0.01),
        'w_gate_up': nrm(25, (L, N_EXPERTS, D, 2 * D_FF), D ** -0.5),
        'b_gate_up': nrm(26, (L, N_EXPERTS, 2 * D_FF), 0.02),
        'w_down': nrm(27, (L, N_EXPERTS, D_FF, D), D_FF ** -0.5),
        'b_down': nrm(28, (L, N_EXPERTS, D), 0.02),
        'final_g': 1.0 + nrm(29, (D,), 0.02),
    }


def reference(x, c, ctx, c_ctx, w_mod, b_mod, norm1_g, w_in, q_norm_g, k_norm_g,
              dn_conv_w, dn_a_log, dn_dt_bias, dn_norm_g, conf_dw_w, conf_dw_b, conf_ln_g, conf_ln_b,
              w_att_o, w_dn_o, w_conf_o, w_out, norm2_g, w_router, b_router,
              w_gate_up, b_gate_up, w_down, b_down, final_g):
    b, s, d = x.shape
    n_ctx = ctx.shape[1]
    rows = s // GRID_W
    cos, sin = axial_rope_tables(rows)
    xc = ctx
    silu_c = jax.nn.silu(c)[:, None, :]
    silu_cc = jax.nn.silu(c_ctx)
    s0 = jnp.zeros((b, DN_H, DN_DK, DN_DV), jnp.float32)
    for l in range(DEPTH):
        ctx_needed = l < DEPTH - 1
        mx = jnp.split(silu_c @ w_mod[l] + b_mod[l], N_MOD, axis=-1)
        mc = jnp.split(silu_cc @ w_mod[l] + b_mod[l], N_MOD, axis=-1)

        hx = modulate(rms_norm(x, norm1_g[l]), mx[0], mx[1])
        hc = modulate(rms_norm(xc, norm1_g[l]), mc[0], mc[1])
        px = split_in(hx @ w_in[l])
        pc = split_in(hc @ w_in[l])

        qx, kx, vx = att_qkv(px, q_norm_g[l], k_norm_g[l])
        qx = apply_rope(qx, cos, sin)
        kx = apply_rope(kx, cos, sin)
        qc, kc, vc = att_qkv(pc, q_norm_g[l], k_norm_g[l])
        att_x = attend_latent(qx, jnp.concatenate([kc, kx], axis=1), jnp.concatenate([vc, vx], axis=1))

        qdc, kdc, vdc, gdc, bdc = dn_stream(pc, dn_conv_w[l], dn_a_log[l], dn_dt_bias[l])
        qdx, kdx, vdx, gdx, bdx = dn_stream(px, dn_conv_w[l], dn_a_log[l], dn_dt_bias[l])
        o_c, s_f, s_b = dn_bidir(qdc, kdc, vdc, gdc, bdc, s0, s0)
        o_x, _, _ = dn_bidir(qdx, kdx, vdx, gdx, bdx, s_f, s_b)

        y_mix_x = merge(att_x @ w_att_o[l],
                        dn_out(o_x, px[8], dn_norm_g[l]) @ w_dn_o[l],
                        conformer(px[9], conf_dw_w[l], conf_dw_b[l], conf_ln_g[l], conf_ln_b[l], w_conf_o[l]),
                        px[10], w_out[l])
        x = x + mx[2] * y_mix_x
        if ctx_needed:
            att_c = gqa_block(qc.reshape(b, n_ctx, ATT_HKV, ATT_GROUP, ATT_DH), kc, vc).reshape(b, n_ctx, ATT_HQ * ATT_DH)
            y_mix_c = merge(att_c @ w_att_o[l],
                            dn_out(o_c, pc[8], dn_norm_g[l]) @ w_dn_o[l],
                            conformer(pc[9], conf_dw_w[l], conf_dw_b[l], conf_ln_g[l], conf_ln_b[l], w_conf_o[l]),
                            pc[10], w_out[l])
            xc = xc + mc[2] * y_mix_c

        hx2 = modulate(rms_norm(x, norm2_g[l]), mx[3], mx[4]).reshape(b * s, d)
        if ctx_needed:
            hc2 = modulate(rms_norm(xc, norm2_g[l]), mc[3], mc[4]).reshape(b * n_ctx, d)
            y = moe(jnp.concatenate([hx2, hc2], axis=0), w_router[l], b_router[l],
                    w_gate_up[l], b_gate_up[l], w_down[l], b_down[l])
            x = x + mx[5] * y[:b * s].reshape(b, s, d)
            xc = xc + mc[5] * y[b * s:].reshape(b, n_ctx, d)
        else:
            y = moe(hx2, w_router[l], b_router[l], w_gate_up[l], b_gate_up[l], w_down[l], b_down[l])
            x = x + mx[5] * y.reshape(b, s, d)
    return rms_norm(x, final_g)
```

```python
import os
import numpy as np
import concourse.bass as bass
import concourse.mybir as mybir
from concourse.bass_utils import run_bass_kernel_spmd

F32 = mybir.dt.float32
BF16 = mybir.dt.bfloat16
AF = mybir.ActivationFunctionType
ALU = mybir.AluOpType
AX = mybir.AxisListType

L_ = 4
D = 1024
NCTX = 256
S = 2048
T = NCTX + S
NT = T // 128
P_IN = 6928
EPS = 1e-6
NE = 32
NR = 1024 + 1024 + 768 + 128 + 32 + 16
NCOL = 48 + 124 + 12 + 512
GROUPS = [(0, 256)] + [(256 + 512 * i, 512) for i in range(4)]


class Res:
    __slots__ = ("w", "r")

    def __init__(self):
        self.w = None
        self.r = []


def RL(n):
    return [Res() for _ in range(n)]


class Instr:
    __slots__ = ("eng", "fn", "deps", "need", "val", "dma", "sem")

    def __init__(self, eng, fn, deps, dma=False):
        self.eng, self.fn, self.deps, self.dma = eng, fn, deps, dma
        self.need = dma
        self.val = None
        self.sem = None


class Prog:
    NRING = 8

    def __init__(self, nc):
        self.nc = nc
        self.E = {"pe": nc.tensor, "act": nc.scalar, "dve": nc.vector, "pool": nc.gpsimd, "sp": nc.sync}
        self.ins = []
        self.last = {}
        self.dmas = []

    def op(self, eng, fn, reads=(), writes=(), dma=False):
        deps = []
        for r in reads:
            if r.w is not None:
                deps.append(r.w)
        for w in writes:
            if w.w is not None:
                deps.append(w.w)
            deps.extend(w.r)
        if eng == "pe" and not dma:
            deps = [d for d in deps if not (d.eng == "pe" and not d.dma)]
        i = Instr(eng, fn, deps, dma)
        for d in deps:
            d.need = True
        for r in reads:
            r.r.append(i)
        for w in writes:
            w.w = i
            w.r = []
        self.ins.append(i)
        if dma:
            self.dmas.append(i)
        else:
            self.last[eng] = i
        return i

    def dma(self, q, out, in_, reads=(), writes=()):
        return self.op(q, lambda: self.E[q].dma_start(out=out, in_=in_), reads, writes, dma=True)

    def barrier(self):
        lasts = list(self.last.values())
        for e in self.E:
            deps = list(lasts) + list(self.dmas)
            for d in deps:
                d.need = True
            self.ins.append(Instr(e, None, deps))
        self.dmas = []

    def emit(self):
        nc = self.nc
        sem = {e: nc.alloc_semaphore("s_" + e) for e in ("pe", "act", "dve", "pool")}
        ring = {q: [nc.alloc_semaphore("r_%s%d" % (q, k)) for k in range(self.NRING)] for q in ("sp", "pool")}
        cnt = {e: 0 for e in sem}
        hist = {q: [0] * self.NRING for q in ring}
        ndma = {q: 0 for q in ring}
        waited = {e: {} for e in self.E}
        for i in self.ins:
            eng = self.E[i.eng]
            need = {}
            for d in i.deps:
                key = id(d.sem)
                if key not in need or need[key][1] < d.val:
                    need[key] = (d.sem, d.val)
            if i.dma:
                k = ndma[i.eng] % self.NRING
                rs = ring[i.eng][k]
                if hist[i.eng][k]:
                    key = id(rs)
                    if key not in need or need[key][1] < hist[i.eng][k]:
                        need[key] = (rs, hist[i.eng][k])
            wt = waited[i.eng]
            for key, (ws, wv) in need.items():
                if wt.get(key, 0) < wv:
                    eng.wait_ge(ws, wv)
                    wt[key] = wv
            if i.fn is None:
                continue
            bi = i.fn()
            if i.dma:
                hist[i.eng][k] += 16
                bi.then_inc(rs, 16)
                i.sem, i.val = rs, hist[i.eng][k]
                ndma[i.eng] += 1
            elif i.need:
                cnt[i.eng] += 1
                bi.then_inc(sem[i.eng], 1)
                i.sem, i.val = sem[i.eng], cnt[i.eng]
        sp = self.E["sp"]
        for q in ring:
            for k in range(self.NRING):
                if hist[q][k]:
                    sp.wait_ge(ring[q][k], hist[q][k])


TILES = []
CUT = int(os.environ.get('K_CUT', '99'))
OFFS = {}
def build(n_layers=L_, dbg=None, stop=None):
    nc = bass.Bass("TRN2", target_bir_lowering=False)
    P = Prog(nc)
    dbg = dbg or {}

    def din(name, shape):
        return nc.dram_tensor(name, list(shape), F32, kind="ExternalInput").ap()

    xin = din("xin", [T, D])
    cvec = din("cvec", [128, 8, 33])
    bmod2 = din("bmod2", [L_, 33, 6 * D])
    rowp = din("rowp", [L_, 128, NR])
    colp = din("colp", [L_, 128, NCOL])
    finalg = din("finalg", [128, D])
    rope = din("rope", [128, 2, NT, 32])
    cmat = din("cmat", [128, 7, 128])
    cmask = din("cmask", [128, 2])
    w_mod = din("w_mod", [L_, D, 6 * D])
    w_in = din("w_in", [L_, D, P_IN])
    w_att_o = din("w_att_o", [L_, 512, D])
    w_dn_o = din("w_dn_o", [L_, 512, D])
    w_conf_o = din("w_conf_o", [L_, 512, D])
    w_out = din("w_out", [L_, D, D])
    w_router = din("w_router", [L_, D, NE])
    w_gate_up = din("w_gate_up", [L_, NE, D, 2 * D] if stop is None else [1, 1, D, 2 * D])
    w_down = din("w_down", [L_, NE, D, D] if stop is None else [1, 1, D, D])
    b_down = din("b_down", [L_, NE, D])
    out = nc.dram_tensor("out", [S, D], F32, kind="ExternalOutput").ap()
    X = nc.dram_tensor("xres", [T, D], F32, kind="Internal").ap()
    MODD = nc.dram_tensor("modd", [L_, 2, 6 * D], F32, kind="Internal").ap()
    r_X = RL(NT)
    r_modd = RL(L_)
    dbg_out = {k: nc.dram_tensor("dbg_" + k, list(shp), F32, kind="ExternalOutput").ap() for k, shp in dbg.items()}

    base = (nc.sbuf_base + 63) // 64 * 64
    top = nc.sbuf_top
    cur = [base]
    uid = [0]

    def alloc(shape, dt):
        nb = int(np.prod(shape[1:])) * (4 if dt == F32 else 2)
        nb = (nb + 63) // 64 * 64
        off = cur[0]
        cur[0] += nb
        assert cur[0] <= top, ("SBUF overflow", cur[0], top)
        uid[0] += 1
        h_ = nc.alloc_sbuf_tensor_at("t%d" % uid[0], list(shape), dt, offset=off)
        TILES.append((h_.name, tuple(shape), h_))
        OFFS[h_.name] = off
        return h_

    CM = alloc([128, 7, 128], F32)
    IDB = alloc([128, 128], BF16)
    CMK = alloc([128, 2], F32)
    ROWP = alloc([128, NR], F32)
    COLP = alloc([128, NCOL], F32)
    HT = alloc([128, 8, T], BF16)
    MB = [None] * 4
    G_ = {}
    r_const, r_rowp, r_colp = Res(), Res(), Res()
    r_HT = RL(NT)
    r_MB = RL(4)
    IDF, MSL, MUI, MSU, MLI, MBLK, ONES = [CM[:, i, :] for i in range(7)]
    arena0 = cur[0]

    def arena_reset():
        P.barrier()
        cur[0] = arena0

    PS = [nc.alloc_psum_tensor("ps%d" % i, [128, 512], F32) for i in range(6)]
    PB = [nc.alloc_psum_tensor("pb%d" % i, [128, 1024], BF16) for i in range(2)]
    r_PS = RL(6)
    r_PB = RL(2)

    def mm(o, lhsT, rhs, start, stop, reads, writes):
        P.op("pe", lambda: nc.tensor.matmul(o, lhsT, rhs, start=start, stop=stop), reads, list(writes))

    def tr(o, in_, ident, reads, writes):
        P.op("pe", lambda: nc.tensor.transpose(out=o, in_=in_, identity=ident), reads, writes)

    def act(o, in_, func, reads, writes, **kw):
        P.op("act", lambda: nc.scalar.activation(out=o, in_=in_, func=func, **kw), reads, writes)

    def tt(eng, o, a, b, op, reads, writes):
        P.op(eng, lambda: P.E[eng].tensor_tensor(out=o, in0=a, in1=b, op=op), reads, writes)

    def ts(eng, o, a, s1, s2, op0, op1, reads, writes):
        if op1 is None:
            P.op(eng, lambda: P.E[eng].tensor_scalar(out=o, in0=a, scalar1=s1, scalar2=None, op0=op0), reads, writes)
        else:
            P.op(eng, lambda: P.E[eng].tensor_scalar(out=o, in0=a, scalar1=s1, scalar2=s2, op0=op0, op1=op1), reads, writes)

    def stt(eng, o, a, sc, b, op0, op1, reads, writes):
        P.op(eng, lambda: P.E[eng].scalar_tensor_tensor(out=o, in0=a, scalar=sc, in1=b, op0=op0, op1=op1), reads, writes)

    def cp(eng, o, a, reads, writes):
        if eng == "act":
            act(o, a, AF.Copy, reads, writes)
        else:
            P.op(eng, lambda: P.E[eng].tensor_copy(out=o, in_=a), reads, writes)

    def red(o, in_, reads, writes):
        P.op("dve", lambda: nc.vector.tensor_reduce(out=o, in_=in_, axis=AX.X, op=ALU.add), reads, writes)

    def recip(o, in_, reads, writes):
        P.op("dve", lambda: nc.vector.reciprocal(out=o, in_=in_), reads, writes)

    def vmax(o, in_, reads, writes):
        P.op("dve", lambda: nc.vector.max(out=o, in_=in_), reads, writes)

    def mset(eng, o, val, writes):
        P.op(eng, lambda: P.E[eng].memset(o, val), [], writes)

    def rsqrt_act(o, in_, scale, bias, reads, writes, post_bias=0.0):
        act(o, in_, AF.Ln, reads, writes, scale=scale, bias=bias)
        act(o, o, AF.Exp, writes, writes, scale=-0.5, bias=post_bias)

    def wload(dst, src, res):
        P.dma("pool", dst, src, writes=[res])

    def tiles_of(q0, n):
        return range(q0 // 128, (q0 + n) // 128)

    P.dma("sp", CM[:], cmat, writes=[r_const])
    P.dma("pool", IDB[:], cmat[:, 0, :], writes=[r_const])
    P.dma("sp", CMK[:], cmask, writes=[r_const])

    SC = alloc([128, 8, 33], F32)
    WM = [alloc([128, 8, 512], F32) for _ in range(2)]
    BM = alloc([33, 6 * D], F32)
    MR = [alloc([33, 512], F32) for _ in range(2)]
    r_SC, r_WM, r_BM, r_MR = Res(), RL(2), Res(), RL(2)
    P.dma("sp", SC[:], cvec, writes=[r_SC])
    act(SC[:], SC[:], AF.Silu, [r_SC], [r_SC])
    it = 0
    for l in range(n_layers):
        P.dma("sp", BM[:], bmod2[l], writes=[r_BM])
        for jt in range(12):
            b = it % 2
            it += 1
            P.dma("sp", WM[b][:], w_mod[l, :, jt * 512:(jt + 1) * 512].rearrange("(kc p) n -> p kc n", p=128), writes=[r_WM[b]])
            ps, rps = PS[b], r_PS[b]
            for kc in range(8):
                mm(ps[0:33, :], SC[:, kc, :], WM[b][:, kc, :], kc == 0, kc == 7, [r_SC, r_WM[b], r_const], [rps])
            tt("dve", MR[b][:], ps[0:33, :], BM[:, jt * 512:(jt + 1) * 512], ALU.add, [rps, r_BM], [r_MR[b]])
            if jt // 2 in (1, 4):
                ts("dve", MR[b][:], MR[b][:], 1.0, None, ALU.add, None, [r_MR[b]], [r_MR[b]])
            P.dma("sp", MODD[l, 0:1, jt * 512:(jt + 1) * 512], MR[b][0:1, :], reads=[r_MR[b]], writes=[r_modd[l]])
            P.dma("sp", MODD[l, 1:2, jt * 512:(jt + 1) * 512], MR[b][32:33, :], reads=[r_MR[b]], writes=[r_modd[l]])

    if stop == "prologue":
        P.emit(); return nc

    def load_mod(l, slot, seg, row):
        P.dma("sp", MB[slot][:], MODD[l, row:row + 1, seg * D:(seg + 1) * D].to_broadcast([128, D]),
              reads=[r_modd[l]], writes=[r_MB[slot]])

    def norm_phase(l, src, r_src, gofs, seg_shift, seg_scale, router=None):
        if router is None:
            for i_ in range(4):
                MB[i_] = alloc([128, D], F32)
        XT = [alloc([128, D], F32) for _ in range(2)]
        HB = [alloc([128, D], BF16 if router is None else F32) for _ in range(2)]
        JK = alloc([128, D], F32)
        SS = [alloc([128, 1], F32) for _ in range(2)]
        r_XT, r_HB, r_JK, r_SS = RL(2), RL(2), Res(), RL(2)
        for row in range(2):
            load_mod(l, row, seg_scale, row)
            load_mod(l, 2 + row, seg_shift, row)
            tt("dve", MB[row][:], MB[row][:], ROWP[:, gofs:gofs + D], ALU.mult, [r_MB[row], r_rowp], [r_MB[row]])
        for t in range(NT):
            b = t % 2
            row = 1 if t < 2 else 0
            P.dma("sp", XT[b][:], src[t * 128:(t + 1) * 128, :], reads=[r_src[t]], writes=[r_XT[b]])
            act(JK[:], XT[b][:], AF.Square, [r_XT[b]], [r_JK, r_SS[b]], accum_out=SS[b][:])
            rsqrt_act(SS[b][:], SS[b][:], 1.0 / D, EPS, [r_SS[b]], [r_SS[b]])
            stt("dve", XT[b][:], XT[b][:], SS[b][:, 0:1], MB[row][:], ALU.mult, ALU.mult, [r_XT[b], r_SS[b], r_MB[row]], [r_XT[b]])
            tt("pool", HB[b][:], XT[b][:], MB[2 + row][:], ALU.add, [r_XT[b], r_MB[2 + row]], [r_HB[b]])
            if router is None:
                pb, rpb = PB[b], r_PB[b]
                for kc in range(8):
                    tr(pb[:, kc * 128:(kc + 1) * 128], HB[b][:, kc * 128:(kc + 1) * 128], IDB[:], [r_HB[b], r_const], [rpb])
                cp("act", HT[:, :, t * 128:(t + 1) * 128], pb[:].rearrange("p (k n) -> p k n", k=8), [rpb], [r_HT[t]])
            else:
                router(t, b, HB[b], r_HB[b])

    def merge_phase(l, bidx, w_bo, YT, r_YT):
        WG = alloc([128, 8, D], BF16)
        WB = alloc([128, 4, D], BF16)
        SG = [alloc([128, 512], F32) for _ in range(2)]
        TM = [alloc([128, 512], BF16) for _ in range(2)]
        r_WG, r_WB, r_SG, r_TM = Res(), Res(), RL(2), RL(2)
        g0 = 3856 + bidx * D
        wload(WG[:], w_in[l, :, g0:g0 + D].rearrange("(kc p) n -> p kc n", p=128), r_WG)
        wload(WB[:], w_bo[l].rearrange("(kc p) n -> p kc n", p=128), r_WB)
        it = 0
        for (q0, n) in GROUPS:
            tl = list(tiles_of(q0, n))
            for dc in range(8):
                b = it % 2
                it += 1
                pa, rpa, pbb, rpbb = PS[b], r_PS[b], PS[2 + b], r_PS[2 + b]
                for kc in range(8):
                    mm(pa[:, 0:n], WG[:, kc, dc * 128:(dc + 1) * 128], HT[:, kc, q0:q0 + n], kc == 0, kc == 7,
                       [r_WG] + [r_HT[t] for t in tl], [rpa])
                for c in range(4):
                    mm(pbb[:, 0:n], WB[:, c, dc * 128:(dc + 1) * 128], G_['BT'][:, c, q0:q0 + n], c == 0, c == 3,
                       [r_WB] + [G_['r_BT'][t] for t in tl], [rpbb])
                act(SG[b][:, 0:n], pa[:, 0:n], AF.Sigmoid, [rpa], [r_SG[b]])
                ry = [r_YT[t] for t in tl]
                if bidx == 0:
                    tt("dve", YT[:, dc, q0:q0 + n], SG[b][:, 0:n], pbb[:, 0:n], ALU.mult, [r_SG[b], rpbb], ry)
                else:
                    tt("dve", TM[b][:, 0:n], SG[b][:, 0:n], pbb[:, 0:n], ALU.mult, [r_SG[b], rpbb], [r_TM[b]])
                    tt("pool", YT[:, dc, q0:q0 + n], YT[:, dc, q0:q0 + n], TM[b][:, 0:n], ALU.add, [r_TM[b]] + ry, ry)

    def to_BT(SRC, r_SRC):
        for t in range(NT):
            b = t % 2
            pb, rpb = PB[b], r_PB[b]
            for c in range(4):
                tr(pb[:, c * 128:(c + 1) * 128], SRC[:, t, c * 128:(c + 1) * 128], IDB[:], [r_SRC[t], r_const], [rpb])
            cp("act", G_['BT'][:, :, t * 128:(t + 1) * 128], pb[:, 0:512].rearrange("p (k n) -> p k n", k=4), [rpb], [G_['r_BT'][t]])

    for l in range(n_layers):
        src, r_src = (xin, RL(NT)) if l == 0 else (X, r_X)
        arena_reset()
        P.dma("sp", ROWP[:], rowp[l], writes=[r_rowp])
        P.dma("sp", COLP[:], colp[l], writes=[r_colp])
        norm_phase(l, src, r_src, 0, 0, 1)

        if stop == "norm1":
            P.emit(); return nc
        arena_reset()
        BT = alloc([128, 4, T], BF16)
        r_BT = RL(NT)
        G_['BT'], G_['r_BT'] = BT, r_BT
        YT = alloc([128, 8, T], BF16)
        r_YT = RL(NT)
        arena1 = cur[0]
        ROPE = alloc([128, 2, NT, 32], F32)
        P.dma("sp", ROPE[:], rope, writes=[r_const])
        WA = alloc([128, 8, 768], BF16)
        QT = alloc([128, 4, T], BF16)
        KT = alloc([128, 2, T], BF16)
        V = alloc([128, NT, 2, 80], BF16)
        ATT = alloc([128, NT, 512], BF16)
        SQ = alloc([128, 640], F32)
        QN = alloc([128, 768], F32)
        QR = alloc([128, 768], BF16)
        TA = [alloc([128, 384], F32) for _ in range(4)]
        RS = alloc([128, 10], F32)
        PT = [alloc([128, 512], BF16) for _ in range(2)]
        RC = alloc([128, 4], F32)
        r_WA, r_QT, r_KT, r_V, r_ATT = Res(), RL(NT), RL(NT), RL(NT), RL(NT)
        r_SQ, r_QN, r_QR, r_TA, r_RS, r_PT, r_RC = Res(), Res(), Res(), RL(4), Res(), RL(2), Res()
        wload(WA[:], w_in[l, :, 0:768].rearrange("(kc p) n -> p kc n", p=128), r_WA)
        mset("dve", V[:].rearrange("p a b c -> p (a b c)"), 1.0, r_V)
        QKG = ROWP[:, 2048:2816]
        for t in range(NT):
            p0, rp0, p1, rp1 = PS[0], r_PS[0], PS[1], r_PS[1]
            for kc in range(8):
                mm(p0[:], HT[:, kc, t * 128:(t + 1) * 128], WA[:, kc, 0:512], kc == 0, kc == 7, [r_HT[t], r_WA], [rp0])
            for kc in range(8):
                mm(p1[:, 0:256], HT[:, kc, t * 128:(t + 1) * 128], WA[:, kc, 512:768], kc == 0, kc == 7, [r_HT[t], r_WA], [rp1])
            cp("act", V[:, t, :, 0:64], p1[:, 128:256].rearrange("p (g d) -> p g d", g=2), [rp1], [r_V[t]])
            act(SQ[:, 0:512], p0[:], AF.Square, [rp0], [r_SQ])
            act(SQ[:, 512:640], p1[:, 0:128], AF.Square, [rp1], [r_SQ])
            red(RS[:], SQ[:].rearrange("p (h d) -> p h d", d=64), [r_SQ], [r_RS])
            rsqrt_act(RS[:], RS[:], 1.0 / 64, EPS, [r_RS], [r_RS])
            tt("dve", QN[:, 0:512].rearrange("p (h d) -> p h d", d=64), p0[:].rearrange("p (h d) -> p h d", d=64),
               RS[:, 0:8].unsqueeze(2).to_broadcast([128, 8, 64]), ALU.mult, [rp0, r_RS], [r_QN])
            for g_ in range(2):
                for e_ in range(2):
                    c0 = 512 + (2 * g_ + e_) * 64
                    tt("dve", QN[:, c0:c0 + 64], p1[:, g_ * 64:(g_ + 1) * 64], RS[:, 8 + g_:9 + g_].to_broadcast([128, 64]), ALU.mult, [rp1, r_RS], [r_QN])
            tt("pool", QN[:], QN[:], QKG, ALU.mult, [r_QN, r_rowp], [r_QN])
            if stop == "qkv_a":
                continue
            q5 = QN[:].rearrange("p (h a b c) -> p h a b c", h=12, a=2, b=2)
            o5 = QR[:].rearrange("p (h a b c) -> p h a b c", h=12, a=2, b=2)
            x1, x2 = q5[:, :, :, 0, :], q5[:, :, :, 1, :]
            cs = ROPE[:, 0, t, :].rearrange("p (a c) -> p a c", a=2).unsqueeze(1).to_broadcast([128, 12, 2, 16])
            sn = ROPE[:, 1, t, :].rearrange("p (a c) -> p a c", a=2).unsqueeze(1).to_broadcast([128, 12, 2, 16])
            tv = [TA[i][:].rearrange("p (h a c) -> p h a c", h=12, a=2) for i in range(4)]
            tt("dve", tv[0], x1, cs, ALU.mult, [r_QN, r_const], [r_TA[0]])
            tt("dve", tv[1], x2, sn, ALU.mult, [r_QN, r_const], [r_TA[1]])
            tt("dve", tv[2], x1, sn, ALU.mult, [r_QN, r_const], [r_TA[2]])
            tt("dve", tv[3], x2, cs, ALU.mult, [r_QN, r_const], [r_TA[3]])
            tt("dve", o5[:, :, :, 0, :], tv[0], tv[1], ALU.subtract, [r_TA[0], r_TA[1]], [r_QR])
            tt("dve", o5[:, :, :, 1, :], tv[2], tv[3], ALU.add, [r_TA[2], r_TA[3]], [r_QR])
            if stop == "qkv_b":
                continue
            pb, rpb = PB[t % 2], r_PB[t % 2]
            for m_ in range(6):
                tr(pb[:, m_ * 128:(m_ + 1) * 128], QR[:, m_ * 128:(m_ + 1) * 128], IDB[:], [r_QR, r_const], [rpb])
            cp("act", QT[:, :, t * 128:(t + 1) * 128], pb[:, 0:512].rearrange("p (h n) -> p h n", h=4), [rpb], [r_QT[t]])
            cp("act", KT[:, :, t * 128:(t + 1) * 128], pb[:, 512:768].rearrange("p (h n) -> p h n", h=2), [rpb], [r_KT[t]])
        if stop in ("qkv", "qkv_a", "qkv_b"):
            P.emit(); return nc
        it = 0
        for h in range(8):
            g = h // 4
            for (q0, n) in GROUPS:
                tl = list(tiles_of(q0, n))
                nk = 2 if q0 == 0 else NT
                ns = n // 128
                for kt in range(nk):
                    b = it % 2
                    it += 1
                    ps, rps = PS[b], r_PS[b]
                    es = slice((h % 2) * 64, (h % 2) * 64 + 64)
                    mm(ps[:, 0:n], KT[es, g, kt * 128:(kt + 1) * 128], QT[es, h // 2, q0:q0 + n], True, True,
                       [r_KT[kt]] + [r_QT[t] for t in tl], [rps])
                    act(PT[b][:, 0:n], ps[:, 0:n], AF.Exp, [rps], [r_PT[b]], scale=0.125)
                    for s_ in range(ns):
                        mm(PS[2 + s_][:, 0:65], PT[b][:, s_ * 128:(s_ + 1) * 128], V[:, kt, g, 0:65], kt == 0, kt == nk - 1,
                           [r_PT[b], r_V[kt]], [r_PS[2 + s_]])
                for s_ in range(ns):
                    t = tl[s_]
                    recip(RC[:, s_:s_ + 1], PS[2 + s_][:, 64:65], [r_PS[2 + s_]], [r_RC])
                    ts("dve", ATT[:, t, h * 64:(h + 1) * 64], PS[2 + s_][:, 0:64], RC[:, s_:s_ + 1], None, ALU.mult, None,
                       [r_PS[2 + s_], r_RC], [r_ATT[t]])
        to_BT(ATT, r_ATT)
        if "att" in dbg and l == 0:
            DB = alloc([128, NT, 512], F32)
            r_DB = Res()
            cp("dve", DB[:], ATT[:], r_ATT, [r_DB])
            P.dma("sp", dbg_out["att"].rearrange("(t p) n -> p t n", p=128), DB[:], reads=[r_DB])
        P.barrier()
        cur[0] = arena1
        merge_phase(l, 0, w_att_o, YT, r_YT)

        if stop == "attn":
            P.emit(); return nc
        P.barrier()
        cur[0] = arena1
        U = alloc([128, 4, T], BF16)
        arena2 = cur[0]
        WC = alloc([128, 8, D], BF16)
        SG = [alloc([128, 512], F32) for _ in range(2)]
        r_WC, r_U, r_CV, r_SG = Res(), RL(4), RL(4), RL(2)
        wload(WC[:], w_in[l, :, 2832:3856].rearrange("(kc p) n -> p kc n", p=128), r_WC)
        it = 0
        for (q0, n) in GROUPS:
            tl = list(tiles_of(q0, n))
            for c in range(4):
                b = it % 2
                it += 1
                pa, rpa, pbb, rpbb = PS[b], r_PS[b], PS[2 + b], r_PS[2 + b]
                for kc in range(8):
                    mm(pa[:, 0:n], WC[:, kc, c * 128:(c + 1) * 128], HT[:, kc, q0:q0 + n], kc == 0, kc == 7, [r_WC] + [r_HT[t] for t in tl], [rpa])
                for kc in range(8):
                    mm(pbb[:, 0:n], WC[:, kc, 512 + c * 128:512 + (c + 1) * 128], HT[:, kc, q0:q0 + n], kc == 0, kc == 7, [r_WC] + [r_HT[t] for t in tl], [rpbb])
                act(SG[b][:, 0:n], pbb[:, 0:n], AF.Sigmoid, [rpbb], [r_SG[b]])
                tt("dve", U[:, c, q0:q0 + n], pa[:, 0:n], SG[b][:, 0:n], ALU.mult, [rpa, r_SG[b]], [r_U[c]])
        P.barrier()
        cur[0] = arena2
        CV = alloc([128, 4, T], F32)
        CW = COLP[:, 48:172].rearrange("p (c k) -> p c k", c=4)
        for c in range(4):
            eng = "dve"
            ts(eng, CV[:, c, :], U[:, c, :], CW[:, c, 15:16], COLP[:, 172 + c:173 + c], ALU.mult, ALU.add, [r_U[c], r_colp], [r_CV[c]])
            for (s0, s1) in ((0, NCTX), (NCTX, T)):
                for j in range(31):
                    o = j - 15
                    if o == 0:
                        continue
                    a0, a1 = max(s0, s0 - o), min(s1, s1 - o)
                    stt(eng, CV[:, c, a0:a1], U[:, c, a0 + o:a1 + o], CW[:, c, j:j + 1], CV[:, c, a0:a1], ALU.mult, ALU.add,
                        [r_U[c], r_colp, r_CV[c]], [r_CV[c]])
        CS = alloc([128, 4, 512], F32)
        MN = alloc([128, 512], F32)
        VR = alloc([128, 512], F32)
        T1 = [alloc([128, 512], F32) for _ in range(2)]
        r_CS, r_MN, r_VR, r_T1 = Res(), Res(), Res(), RL(2)
        for (q0, n) in GROUPS:
            tl = list(tiles_of(q0, n))
            act(CS[:, :, 0:n], CV[:, :, q0:q0 + n], AF.Square, r_CV, [r_CS])
            for c in range(4):
                mm(PS[0][:, 0:n], ONES, CV[:, c, q0:q0 + n], c == 0, c == 3, [r_const, r_CV[c]], [r_PS[0]])
            for c in range(4):
                mm(PS[1][:, 0:n], ONES, CS[:, c, 0:n], c == 0, c == 3, [r_const, r_CS], [r_PS[1]])
            act(MN[:, 0:n], PS[0][:, 0:n], AF.Copy, [r_PS[0]], [r_MN], scale=1.0 / 512)
            tt("dve", VR[:, 0:n], MN[:, 0:n], MN[:, 0:n], ALU.mult, [r_MN], [r_VR])
            stt("dve", VR[:, 0:n], PS[1][:, 0:n], 1.0 / 512, VR[:, 0:n], ALU.mult, ALU.subtract, [r_PS[1], r_VR], [r_VR])
            rsqrt_act(VR[:, 0:n], VR[:, 0:n], 1.0, EPS, [r_VR], [r_VR])
            for c in range(4):
                b = c % 2
                tt("dve", T1[b][:, 0:n], CV[:, c, q0:q0 + n], MN[:, 0:n], ALU.subtract, [r_CV[c], r_MN], [r_T1[b]])
                tt("pool", T1[b][:, 0:n], T1[b][:, 0:n], VR[:, 0:n], ALU.mult, [r_T1[b], r_VR], [r_T1[b]])
                act(G_['BT'][:, c, q0:q0 + n], T1[b][:, 0:n], AF.Silu, [r_T1[b], r_colp], [G_['r_BT'][t] for t in tl],
                    scale=COLP[:, 176 + c:177 + c], bias=COLP[:, 180 + c:181 + c])
        P.barrier()
        cur[0] = arena1
        merge_phase(l, 2, w_conf_o, YT, r_YT)

        if stop == "conf":
            P.emit(); return nc
        P.barrier()
        cur[0] = arena1
        DNO = alloc([128, NT, 128], BF16)
        ZG = alloc([128, NT, 128], BF16)
        r_DNO, r_ZG = RL(NT), RL(NT)
        WZ = alloc([128, 8, 528], BF16)
        r_WZ = Res()
        wload(WZ[:], w_in[l, :, 2304:2832].rearrange("(kc p) n -> p kc n", p=128), r_WZ)
        BG = alloc([128, NT, 16], F32)
        r_BG = Res()
        for t in range(NT):
            b = t % 2
            for kc in range(8):
                mm(PS[2 + b][:, 0:16], HT[:, kc, t * 128:(t + 1) * 128], WZ[:, kc, 0:16], kc == 0, kc == 7, [r_HT[t], r_WZ], [r_PS[2 + b]])
            cp("act", BG[:, t, :], PS[2 + b][:, 0:16], [r_PS[2 + b]], [r_BG])
        BETA = alloc([128, NT, 8], F32)
        GG = alloc([128, NT, 8], F32)
        GM = alloc([128, NT, 2, 8], F32)
        GC = alloc([128, NT, 8], F32)
        GL = alloc([128, NT, 8], F32)
        EGC = alloc([128, NT, 8], F32)
        KD = alloc([128, NT, 8], F32)
        BE = alloc([128, NT, 8], F32)
        ETOT = alloc([128, NT, 2, 8], F32)
        NEGA = alloc([128, 8], F32)
        r_G = Res()
        act(BETA[:], BG[:, :, 0:8], AF.Sigmoid, [r_BG], [r_G])
        act(NEGA[:], ROWP[:, 2976:2984], AF.Exp, [r_rowp], [r_G])
        ts("dve", NEGA[:], NEGA[:], -1.0, None, ALU.mult, None, [r_G], [r_G])
        tt("dve", GG[:], BG[:, :, 8:16], ROWP[:, 2984:2992].unsqueeze(1).to_broadcast([128, NT, 8]), ALU.add, [r_BG, r_rowp], [r_G])
        act(GG[:], GG[:], AF.Exp, [r_G], [r_G])
        act(GG[:], GG[:], AF.Ln, [r_G], [r_G], bias=1.0)
        tt("dve", GG[:], GG[:], NEGA[:].unsqueeze(1).to_broadcast([128, NT, 8]), ALU.mult, [r_G], [r_G])
        for c in range(2):
            ts("dve", GM[:, :, c, :], GG[:], CMK[:, c:c + 1], None, ALU.mult, None, [r_G, r_const], [r_G])
        for t in range(NT):
            b = t % 2
            ps, rps = PS[b], r_PS[b]
            mm(ps[:, 0:4], MUI, GG[:, t, 0:4], True, True, [r_const, r_G], [rps])
            mm(ps[:, 4:8], MLI, GG[:, t, 4:8], True, True, [r_const, r_G], [rps])
            mm(ps[:, 8:16], MBLK, GG[:, t, :], True, True, [r_const, r_G], [rps])
            mm(ps[:, 16:32], ONES, GM[:, t, :, :].rearrange("p c e -> p (c e)"), True, True, [r_const, r_G], [rps])
            cp("act", GC[:, t, :], ps[:, 0:8], [rps], [r_G])
            cp("act", GL[:, t, :], ps[:, 8:16], [rps], [r_G])
            act(ETOT[:, t, :, :].rearrange("p c e -> p (c e)"), ps[:, 16:32], AF.Exp, [rps], [r_G])
        act(EGC[:], GC[:], AF.Exp, [r_G], [r_G])
        tt("dve", KD[:], GL[:], GC[:], ALU.subtract, [r_G], [r_G])
        act(KD[:], KD[:], AF.Exp, [r_G], [r_G])
        tt("dve", BE[:], BETA[:], EGC[:], ALU.mult, [r_G], [r_G])

        if stop == "dn_a":
            P.emit(); return nc
        WD = alloc([128, 8, 384], BF16)
        r_WD = Res()
        ZC = alloc([128, T], F32)
        CO = alloc([128, T], F32)
        SQD = alloc([128, 512], F32)
        RN = alloc([128, 512], F32)
        QTh = alloc([128, T], BF16)
        KTh = alloc([128, T], BF16)
        VTh = alloc([128, T], BF16)
        KTOK = alloc([128, NT, 128], BF16)
        VTOK = alloc([128, NT, 128], BF16)
        OACC = alloc([128, NT, 128], F32)
        r_ZC, r_CO, r_SQD, r_RN = Res(), Res(), Res(), Res()
        SZ, r_SZ = CO, r_CO
        r_QTh, r_KTh, r_VTh, r_KTOK, r_VTOK, r_OACC = RL(NT), RL(NT), RL(NT), RL(NT), RL(NT), RL(NT)
        def ubuf(shape, dt):
            return [alloc(shape, dt) for _ in range(2)]
        DG, E1, E2, LM, LTm, XA, XTA, TTm = [ubuf([128, 128], F32) for _ in range(8)]
        ITm, QG, KBG, KDEC, WTm, TTb, VB, VNb = [ubuf([128, 128], BF16) for _ in range(8)]
        def alias(h_, nm):
            uid[0] += 1
            return nc.alloc_sbuf_tensor_at("t%d%s" % (uid[0], nm), [128, 128], BF16, offset=OFFS[h_.name])
        ITc = [alias(E1[d_], "itc") for d_ in range(2)]
        KDc = [alias(E2[d_], "kdc") for d_ in range(2)]
        Um = ubuf([128, 128], F32)
        Sf = ubuf([128, 128], F32)
        Sb = ubuf([128, 128], BF16)
        r_u = [dict((k, Res()) for k in ("DG", "E1", "E2", "L", "LT", "XA", "XTA", "XB", "XTB", "TT", "IT", "QG", "KBG", "KDEC",
                                          "WT", "TTb", "VB", "VN", "U", "Sf", "Sb", "ITc", "KDc")) for _ in range(2)]
        CWD = COLP[:, 0:48].rearrange("p (c k) -> p c k", c=12)
        for h in range(4):
            for t in range(NT):
                b = t % 2
                for kc in range(8):
                    mm(PS[b][:, 0:128], HT[:, kc, t * 128:(t + 1) * 128], WZ[:, kc, 16 + h * 128:16 + (h + 1) * 128], kc == 0, kc == 7, [r_HT[t], r_WZ], [r_PS[b]])
                act(ZG[:, t, :], PS[b][:, 0:128], AF.Silu, [r_PS[b]], [r_ZG[t]])
            for j in range(3):
                c0 = 768 + j * 512 + h * 128
                wload(WD[:, :, j * 128:(j + 1) * 128], w_in[l, :, c0:c0 + 128].rearrange("(kc p) n -> p kc n", p=128), r_WD)
            for j in range(3):
                ch = j * 4 + h
                it = 0
                for (q0, n) in GROUPS:
                    b = it % 2
                    it += 1
                    for kc in range(8):
                        mm(PS[b][:, 0:n], WD[:, kc, j * 128:(j + 1) * 128], HT[:, kc, q0:q0 + n], kc == 0, kc == 7,
                           [r_WD] + [r_HT[t] for t in tiles_of(q0, n)], [r_PS[b]])
                    cp("act", ZC[:, q0:q0 + n], PS[b][:, 0:n], [r_PS[b]], [r_ZC])
                ts("dve", CO[:], ZC[:], CWD[:, ch, 1:2], None, ALU.mult, None, [r_ZC, r_colp], [r_CO])
                for (s0, s1) in ((0, NCTX), (NCTX, T)):
                    for k_, o in ((0, -1), (2, 1), (3, 2)):
                        a0, a1 = max(s0, s0 - o), min(s1, s1 - o)
                        stt("dve", CO[:, a0:a1], ZC[:, a0 + o:a1 + o], CWD[:, ch, k_:k_ + 1], CO[:, a0:a1], ALU.mult, ALU.add,
                            [r_ZC, r_colp, r_CO], [r_CO])
                if j == 2:
                    act(VTh[:], CO[:], AF.Silu, [r_CO], r_VTh)
                else:
                    act(CO[:], CO[:], AF.Silu, [r_CO], [r_CO])
                    dst, r_dst = (QTh, r_QTh) if j == 0 else (KTh, r_KTh)
                    for (q0, n) in GROUPS:
                        act(SQD[:, 0:n], SZ[:, q0:q0 + n], AF.Square, [r_SZ], [r_SQD])
                        mm(PS[2][:, 0:n], ONES, SQD[:, 0:n], True, True, [r_const, r_SQD], [r_PS[2]])
                        rsqrt_act(RN[:, 0:n], PS[2][:, 0:n], 1.0, EPS, [r_PS[2]], [r_RN],
                                  post_bias=(-0.5 * float(np.log(128.0)) if j == 0 else 0.0))
                        tt("dve", dst[:, q0:q0 + n], SZ[:, q0:q0 + n], RN[:, 0:n], ALU.mult, [r_SZ, r_RN], [r_dst[t] for t in tiles_of(q0, n)])
            for t in range(NT):
                b = t % 2
                tr(PB[b][:, 0:128], KTh[:, t * 128:(t + 1) * 128], IDB[:], [r_KTh[t], r_const], [r_PB[b]])
                tr(PB[b][:, 128:256], VTh[:, t * 128:(t + 1) * 128], IDB[:], [r_VTh[t], r_const], [r_PB[b]])
                cp("act", KTOK[:, t, :], PB[b][:, 0:128], [r_PB[b]], [r_KTOK[t]])
                cp("act", VTOK[:, t, :], PB[b][:, 128:256], [r_PB[b]], [r_VTOK[t]])
            if stop == "dn_b":
                P.emit(); return nc
            order = [list(range(NT)), [1, 0] + list(range(NT - 1, 1, -1))]
            mset("dve", OACC[:].rearrange("p a b -> p (a b)"), 0.0, r_OACC)
            for d in range(2):
                mset("dve", Sf[d][:], 0.0, [r_u[d]["Sf"]])
                mset("dve", Sb[d][:], 0.0, [r_u[d]["Sb"]])
            for step in range(NT):
                if stop == "dn_s1" and step >= 1:
                    break
                for d in range(2):
                    if stop == "dn_s1" and d == 1:
                        continue
                    t = order[d][step]
                    col = d * 4 + h
                    R = r_u[d]
                    tsl = slice(t * 128, (t + 1) * 128)
                    pG, rG, pQ, rQ, pD, rD = PS[3 * d], r_PS[3 * d], PS[3 * d + 1], r_PS[3 * d + 1], PS[3 * d + 2], r_PS[3 * d + 2]
                    gc = GC[:, t, col:col + 1]
                    mm(pG[:, 0:128], KTh[:, tsl], KTh[:, tsl], True, True, [r_KTh[t]], [rG])
                    mm(pG[:, 128:256], KTh[:, tsl], QTh[:, tsl], True, True, [r_KTh[t], r_QTh[t]], [rG])
                    if CUT <= 1:
                        continue
                    ts("dve", DG[d][:], IDF, gc, None, ALU.mult, None, [r_const, r_G], [R["DG"]])
                    mm(pG[:, 256:384], ONES, DG[d][:], True, True, [r_const, R["DG"]], [rG])
                    dgp = pG[:, 256:384]
                    if CUT <= 2:
                        continue
                    ts("dve", E1[d][:], dgp, gc, 0.0, ALU.subtract, ALU.max, [rG, r_G], [R["E1"]])
                    ts("dve", E2[d][:], dgp, gc, 0.0, ALU.subtract, ALU.min, [rG, r_G], [R["E2"]])
                    act(E1[d][:], E1[d][:], AF.Exp, [R["E1"]], [R["E1"]], scale=-1.0)
                    act(E2[d][:], E2[d][:], AF.Exp, [R["E2"]], [R["E2"]])
                    act(QG[d][:], dgp, AF.Exp, [rG], [R["QG"]])
                    if CUT <= 3:
                        continue
                    tt("dve", E1[d][:], E1[d][:], MSL if d == 0 else MSU, ALU.mult, [R["E1"], r_const], [R["E1"]])
                    tt("dve", E2[d][:], E2[d][:], MUI if d == 0 else MLI, ALU.mult, [R["E2"], r_const], [R["E2"]])
                    stt("dve", LM[d][:], pG[:, 0:128], BETA[:, t, col:col + 1], E1[d][:], ALU.mult, ALU.mult, [rG, r_G, R["E1"]], [R["L"]])
                    tt("dve", ITm[d][:], pG[:, 128:256], E2[d][:], ALU.mult, [rG, R["E2"]], [R["IT"]])
                    tt("dve", QG[d][:], QG[d][:], QTh[:, tsl], ALU.mult, [R["QG"], r_QTh[t]], [R["QG"]])
                    if CUT <= 4:
                        continue
                    ts("dve", KBG[d][:], KTOK[:, t, :], BE[:, t, col:col + 1], None, ALU.mult, None, [r_KTOK[t], r_G], [R["KBG"]])
                    ts("dve", KDEC[d][:], KTOK[:, t, :], KD[:, t, col:col + 1], None, ALU.mult, None, [r_KTOK[t], r_G], [R["KDEC"]])
                    ts("dve", VB[d][:], VTOK[:, t, :], BETA[:, t, col:col + 1], None, ALU.mult, None, [r_VTOK[t], r_G], [R["VB"]])
                    if CUT <= 5:
                        continue
                    tr(pQ[:, 0:128], LM[d][:], IDF, [R["L"], r_const], [rQ])
                    cp("act", LTm[d][:], pQ[:, 0:128], [rQ], [R["LT"]])
                    tt("dve", TTm[d][:], IDF, LTm[d][:], ALU.subtract, [r_const, R["LT"]], [R["TT"]])
                    if CUT <= 6:
                        continue
                    Xc, XTc, rX, rXT = LM[d], LTm[d], R["L"], R["LT"]
                    nxt = [(XA[d], XTA[d], R["XA"], R["XTA"]), (LM[d], LTm[d], R["L"], R["LT"])]
                    for k_ in range(5):
                        Xn, XTn, rXn, rXTn = nxt[k_ % 2]
                        mm(pQ[:, 128:256], XTc[:], Xc[:], True, True, [rX, rXT], [rQ])
                        if k_ < 4:
                            mm(pQ[:, 256:384], Xc[:], XTc[:], True, True, [rX, rXT], [rQ])
                        cp("act", Xn[:], pQ[:, 128:256], [rQ], [rXn])
                        if k_ < 4:
                            cp("act", XTn[:], pQ[:, 256:384], [rQ], [rXTn])
                        mm(pQ[:, 384:512], Xn[:], TTm[d][:], True, True, [rXn, R["TT"]], [rQ])
                        tt("dve", TTm[d][:], TTm[d][:], pQ[:, 384:512], ALU.add, [R["TT"], rQ], [R["TT"]])
                        Xc, XTc, rX, rXT = Xn, XTn, rXn, rXTn
                    if stop == "dn_c" or CUT <= 7:
                        continue
                    cp("act", TTb[d][:], TTm[d][:], [R["TT"]], [R["TTb"]])
                    mm(pD[:, 0:128], TTb[d][:], VB[d][:], True, True, [R["TTb"], R["VB"]], [rD])
                    mm(pD[:, 128:256], KBG[d][:], TTb[d][:], True, True, [R["TTb"], R["KBG"]], [rD])
                    cp("act", Um[d][:], pD[:, 0:128], [rD], [R["U"]])
                    cp("act", WTm[d][:], pD[:, 128:256], [rD], [R["WT"]])
                    if CUT <= 8:
                        continue
                    for c in ((0, 1) if d == 0 else (1, 0)):
                        ts("dve", ITc[d][:], ITm[d][:], CMK[:, c:c + 1], None, ALU.mult, None, [R["IT"], r_const], [R["E1"]])
                        ts("dve", KDc[d][:], KDEC[d][:], CMK[:, c:c + 1], None, ALU.mult, None, [R["KDEC"], r_const], [R["E2"]])
                        mm(pD[:, 256:384], WTm[d][:], Sb[d][:], True, True, [R["WT"], R["Sb"]], [rD])
                        tt("dve", VNb[d][:], Um[d][:], pD[:, 256:384], ALU.subtract, [R["U"], rD], [R["VN"]])
                        mm(pD[:, 384:512], QG[d][:], Sb[d][:], True, False, [R["QG"], R["Sb"]], [rD])
                        mm(pD[:, 384:512], ITc[d][:], VNb[d][:], False, True, [R["E1"], R["VN"]], [rD])
                        mm(pG[:, 384:512], KDc[d][:], VNb[d][:], True, True, [R["E2"], R["VN"]], [rG])
                        stt("dve", OACC[:, t, :], pD[:, 384:512], CMK[:, c:c + 1], OACC[:, t, :], ALU.mult, ALU.add, [rD, r_const, r_OACC[t]], [r_OACC[t]])
                        stt("dve", Sf[d][:], Sf[d][:], ETOT[:, t, c, col:col + 1], pG[:, 384:512], ALU.mult, ALU.add, [R["Sf"], r_G, rG], [R["Sf"]])
                        cp("act", Sb[d][:], Sf[d][:], [R["Sf"]], [R["Sb"]])
            if stop in ("dn_c", "dn_d", "dn_s1"):
                P.emit(); return nc
            OSQ = ZC[:, 0:NT * 128].rearrange("p (t n) -> p t n", n=128)
            ORS = RN[:, 0:NT]
            r_OSQ = r_ZC
            act(OSQ, OACC[:], AF.Square, r_OACC, [r_OSQ])
            red(ORS, OSQ, [r_OSQ], [r_RN])
            rsqrt_act(ORS, ORS, 1.0 / 128, EPS, [r_RN], [r_RN])
            tt("dve", OSQ, OACC[:], ORS.unsqueeze(2).to_broadcast([128, NT, 128]), ALU.mult, r_OACC + [r_RN], [r_OSQ])
            tt("pool", OSQ, OSQ, ROWP[:, 2816:2944].unsqueeze(1).to_broadcast([128, NT, 128]), ALU.mult, [r_OSQ, r_rowp], [r_OSQ])
            tt("dve", DNO[:], OSQ, ZG[:], ALU.mult, [r_OSQ] + r_ZG, r_DNO)
            for t in range(NT):
                b = t % 2
                tr(PB[b][:, 0:128], DNO[:, t, :], IDB[:], [r_DNO[t], r_const], [r_PB[b]])
                cp("act", BT[:, h, t * 128:(t + 1) * 128], PB[b][:, 0:128], [r_PB[b]], [r_BT[t]])
        if "dno" in dbg and l == 0:
            P.barrier()
            DB = alloc([128, 4, 512], F32)
            r_DB = Res()
            cp("dve", DB[:], BT[:, :, 256:768], r_BT, [r_DB])
            P.dma("sp", dbg_out["dno"].rearrange("(c p) n -> p c n", p=128), DB[:], reads=[r_DB])
        P.barrier()
        cur[0] = arena1
        merge_phase(l, 1, w_dn_o, YT, r_YT)

        if stop == "dn":
            P.emit(); return nc
        P.barrier()
        cur[0] = arena1
        WO = alloc([128, 8, D], BF16)
        r_WO = Res()
        wload(WO[:], w_out[l].rearrange("(kc p) n -> p kc n", p=128), r_WO)
        XT = [alloc([128, D], F32) for _ in range(2)]
        TM = [alloc([128, D], F32) for _ in range(2)]
        r_XT, r_TM = RL(2), RL(2)
        MB[0] = alloc([128, D], F32)
        MB[1] = alloc([128, D], F32)
        load_mod(l, 0, 2, 0)
        load_mod(l, 1, 2, 1)
        for t in range(NT):
            b = t % 2
            row = 1 if t < 2 else 0
            P.dma("sp", XT[b][:], src[t * 128:(t + 1) * 128, :], reads=[r_src[t]], writes=[r_XT[b]])
            for n_ in range(2):
                ps, rps = PS[2 * b + n_], r_PS[2 * b + n_]
                for kc in range(8):
                    mm(ps[:], YT[:, kc, t * 128:(t + 1) * 128], WO[:, kc, n_ * 512:(n_ + 1) * 512], kc == 0, kc == 7, [r_YT[t], r_WO], [rps])
                tt("dve", TM[b][:, n_ * 512:(n_ + 1) * 512], ps[:], MB[row][:, n_ * 512:(n_ + 1) * 512], ALU.mult, [rps, r_MB[row]], [r_TM[b]])
            tt("pool", XT[b][:], XT[b][:], TM[b][:], ALU.add, [r_XT[b], r_TM[b]], [r_XT[b]])
            P.dma("sp", X[t * 128:(t + 1) * 128, :], XT[b][:], reads=[r_XT[b]], writes=[r_X[t]])
        if "x1" in dbg and l == 0:
            P.barrier()
            P.dma("sp", dbg_out["x1"], X, reads=r_X)

        if stop == "oproj":
            P.emit(); return nc
        arena_reset()
        COMB = alloc([128, NT, NE], F32)
        COMBT = alloc([32, NT, 128], F32)
        BDN = alloc([32, D], F32)
        for i_ in range(4):
            MB[i_] = alloc([128, D], F32)
        arena3 = cur[0]
        WR = alloc([128, 8, NE], F32)
        HT32 = [alloc([128, 8, 128], F32) for _ in range(2)]
        LG = alloc([128, NE], F32)
        MX8 = alloc([128, 8], F32)
        MSK = alloc([128, NE], F32)
        SM = alloc([128, 2], F32)
        r_WR, r_HT32, r_COMB, r_COMBT, r_BDN, r_LG, r_MX8, r_MSK, r_SM = Res(), RL(2), RL(NT), RL(NT), Res(), Res(), Res(), Res(), Res()
        P.dma("sp", WR[:], w_router[l].rearrange("(kc p) n -> p kc n", p=128), writes=[r_WR])
        P.dma("sp", BDN[:], b_down[l], writes=[r_BDN])

        def router(t, b, HBt, r_HBt):
            for half in range(2):
                ps, rps = PS[2 * b + half], r_PS[2 * b + half]
                for k4 in range(4):
                    kc = half * 4 + k4
                    tr(ps[:, k4 * 128:(k4 + 1) * 128], HBt[:, kc * 128:(kc + 1) * 128], IDF, [r_HBt, r_const], [rps])
                cp("act", HT32[b][:, half * 4:(half + 1) * 4, :], ps[:].rearrange("p (k n) -> p k n", k=4), [rps], [r_HT32[b]])
                cp("act", HT[:, half * 4:(half + 1) * 4, t * 128:(t + 1) * 128], ps[:].rearrange("p (k n) -> p k n", k=4), [rps], [r_HT[t]])
            ps, rps = PS[4 + b], r_PS[4 + b]
            for kc in range(8):
                mm(ps[:, 0:NE], HT32[b][:, kc, :], WR[:, kc, :], kc == 0, kc == 7, [r_HT32[b], r_WR], [rps])
            tt("dve", LG[:], ps[:, 0:NE], ROWP[:, 2944:2976], ALU.add, [rps, r_rowp], [r_LG])
            vmax(MX8[:], LG[:], [r_LG], [r_MX8])
            tt("dve", MSK[:], LG[:], MX8[:, 3:4].to_broadcast([128, NE]), ALU.is_ge, [r_LG, r_MX8], [r_MSK])
            ts("dve", SM[:, 0:1], MX8[:, 0:1], -1.0, None, ALU.mult, None, [r_MX8], [r_SM])
            act(LG[:], LG[:], AF.Exp, [r_LG, r_SM], [r_LG], bias=SM[:, 0:1])
            tt("dve", LG[:], LG[:], MSK[:], ALU.mult, [r_LG, r_MSK], [r_LG])
            red(SM[:, 1:2], LG[:], [r_LG], [r_SM])
            recip(SM[:, 1:2], SM[:, 1:2], [r_SM], [r_SM])
            ts("dve", COMB[:, t, :], LG[:], SM[:, 1:2], None, ALU.mult, None, [r_LG, r_SM], [r_COMB[t]])
            tr(ps[0:32, 128:256], COMB[:, t, :], IDF, [r_COMB[t], r_const], [rps])
            cp("act", COMBT[:, t, :], ps[0:32, 128:256], [rps], [r_COMBT[t]])

        norm_phase(l, X, r_X, 1024, 3, 4, router=router)
        if "comb" in dbg and l == 0:
            P.dma("sp", dbg_out["comb"].rearrange("(t p) n -> p t n", p=128), COMB[:], reads=r_COMB)

        if stop == "norm2":
            P.emit(); return nc
        P.barrier()
        cur[0] = arena3
        SM = alloc([128, 2], F32)
        r_SM = Res()
        WGU = [alloc([128, 8, 2 * D], BF16)]
        WDN = [alloc([128, 8, D], BF16)]
        ACC = alloc([128, 9, D], F32)
        AT = [alloc([128, 8, 384], BF16) for _ in range(3)]
        GCl = [alloc([128, 384], F32) for _ in range(2)]
        SGm = [alloc([128, 384], F32) for _ in range(2)]
        UC = [alloc([128, 384], F32) for _ in range(2)]
        XT = [alloc([128, D], F32) for _ in range(2)]
        r_WGU, r_WDN, r_ACC, r_AT, r_GC, r_SGm, r_UC, r_XT = RL(1), RL(1), RL(9), RL(3), RL(2), RL(2), RL(2), RL(2)
        BGU = COLP[:, 184:696].rearrange("p (e c) -> p e c", e=NE)
        load_mod(l, 0, 5, 0)
        load_mod(l, 1, 5, 1)
        last = (l == n_layers - 1)
        if last:
            P.dma("sp", MB[2][:], finalg, writes=[r_MB[2]])
        ei = 0
        for half in range(2):
            tiles = list(range(half * 9, half * 9 + 9))
            for i, t in enumerate(tiles):
                for n_ in range(2):
                    ps, rps = PS[n_], r_PS[n_]
                    mm(ps[:], COMBT[:, t, :], BDN[:, n_ * 512:(n_ + 1) * 512], True, True, [r_COMBT[t], r_BDN], [rps])
                    cp("act", ACC[:, i, n_ * 512:(n_ + 1) * 512], ps[:], [rps], [r_ACC[i]])
            for e in range(NE):
                wb = 0
                wload(WGU[wb][:], w_gate_up[l, e].rearrange("(kc p) n -> p kc n", p=128), r_WGU[wb])
                wload(WDN[wb][:], w_down[l, e].rearrange("(kc p) n -> p kc n", p=128), r_WDN[wb])
                for tg in range(3):
                    q0 = (half * 9 + tg * 3) * 128
                    n = 384
                    tl = tiles[tg * 3:tg * 3 + 3]
                    ab = tg
                    for j in range(8):
                        b = j % 2
                        pg, rpg, pu, rpu = PS[b], r_PS[b], PS[2 + b], r_PS[2 + b]
                        for kc in range(8):
                            mm(pg[:, 0:n], WGU[wb][:, kc, j * 128:(j + 1) * 128], HT[:, kc, q0:q0 + n], kc == 0, kc == 7,
                               [r_WGU[wb]] + [r_HT[t] for t in tl], [rpg])
                        for kc in range(8):
                            mm(pu[:, 0:n], WGU[wb][:, kc, D + j * 128:D + (j + 1) * 128], HT[:, kc, q0:q0 + n], kc == 0, kc == 7,
                               [r_WGU[wb]] + [r_HT[t] for t in tl], [rpu])
                        ts("dve", GCl[b][:], pg[:, 0:n], BGU[:, e, j:j + 1], 7.0, ALU.add, ALU.min, [rpg, r_colp], [r_GC[b]])
                        act(SGm[b][:], GCl[b][:], AF.Sigmoid, [r_GC[b]], [r_SGm[b]], scale=1.702)
                        ts("dve", UC[b][:], pu[:, 0:n], BGU[:, e, 8 + j:9 + j], 7.0, ALU.add, ALU.min, [rpu, r_colp], [r_UC[b]])
                        ts("pool", UC[b][:], UC[b][:], -7.0, 1.0, ALU.max, ALU.add, [r_UC[b]], [r_UC[b]])
                        tt("pool", GCl[b][:], GCl[b][:], SGm[b][:], ALU.mult, [r_GC[b], r_SGm[b]], [r_GC[b]])
                        tt("pool", AT[ab][:, j, :], GCl[b][:], UC[b][:], ALU.mult, [r_GC[b], r_UC[b]], [r_AT[ab]])
                for tg in range(3):
                    tl = tiles[tg * 3:tg * 3 + 3]
                    ab = tg
                    for s_ in range(3):
                        t = tl[s_]
                        i = t - half * 9
                        for n_ in range(2):
                            ps, rps = PS[4 + n_], r_PS[4 + n_]
                            for j in range(8):
                                mm(ps[:], AT[ab][:, j, s_ * 128:(s_ + 1) * 128], WDN[wb][:, j, n_ * 512:(n_ + 1) * 512], j == 0, j == 7,
                                   [r_AT[ab], r_WDN[wb]], [rps])
                            stt("dve", ACC[:, i, n_ * 512:(n_ + 1) * 512], ps[:], COMB[:, t, e:e + 1], ACC[:, i, n_ * 512:(n_ + 1) * 512],
                                ALU.mult, ALU.add, [rps, r_COMB[t], r_ACC[i]], [r_ACC[i]])
            for i, t in enumerate(tiles):
                b = i % 2
                row = 1 if t < 2 else 0
                if last and t < 2:
                    continue
                P.dma("sp", XT[b][:], X[t * 128:(t + 1) * 128, :], reads=[r_X[t]], writes=[r_XT[b]])
                tt("dve", ACC[:, i, :], ACC[:, i, :], MB[row][:], ALU.mult, [r_ACC[i], r_MB[row]], [r_ACC[i]])
                tt("pool", XT[b][:], XT[b][:], ACC[:, i, :], ALU.add, [r_XT[b], r_ACC[i]], [r_XT[b]])
                if not last:
                    P.dma("sp", X[t * 128:(t + 1) * 128, :], XT[b][:], reads=[r_XT[b]], writes=[r_X[t]])
                else:
                    act(ACC[:, i, :], XT[b][:], AF.Square, [r_XT[b]], [r_ACC[i], r_SM], accum_out=SM[:, 0:1])
                    rsqrt_act(SM[:, 0:1], SM[:, 0:1], 1.0 / D, EPS, [r_SM], [r_SM])
                    stt("dve", XT[b][:], XT[b][:], SM[:, 0:1], MB[2][:], ALU.mult, ALU.mult, [r_XT[b], r_SM, r_MB[2]], [r_XT[b]])
                    P.dma("sp", out[(t - 2) * 128:(t - 1) * 128, :], XT[b][:], reads=[r_XT[b]])
    P.emit()
    return nc


def _consts():
    p = np.arange(128)
    same = (p[:, None] // 64) == (p[None, :] // 64)
    cm = np.zeros((128, 7, 128), np.float32)
    cm[:, 0] = np.eye(128)
    cm[:, 1] = same & (p[None, :] < p[:, None])
    cm[:, 2] = same & (p[None, :] >= p[:, None])
    cm[:, 3] = same & (p[None, :] > p[:, None])
    cm[:, 4] = same & (p[None, :] <= p[:, None])
    cm[:, 5] = same
    cm[:, 6] = 1.0
    cmask = np.stack([(p < 64), (p >= 64)], 1).astype(np.float32)
    inv = 10000.0 ** (-np.arange(16, dtype=np.float32) / 16)
    pos = np.arange(S)
    r, c = (pos // 64).astype(np.float32), (pos % 64).astype(np.float32)
    ang = np.stack([r[:, None] * inv, c[:, None] * inv], 1).astype(np.float32)
    cos = np.concatenate([np.ones((NCTX, 2, 16), np.float32), np.cos(ang)], 0).reshape(NT, 128, 32)
    sin = np.concatenate([np.zeros((NCTX, 2, 16), np.float32), np.sin(ang)], 0).reshape(NT, 128, 32)
    rope = np.stack([cos.transpose(1, 0, 2), sin.transpose(1, 0, 2)], 1).astype(np.float32)
    return cm, cmask, np.ascontiguousarray(rope)


def _prep(inp):
    f = lambda a: np.ascontiguousarray(np.asarray(a, dtype=np.float32))
    cm, cmask, rope = _consts()
    rep = lambda v: np.broadcast_to(np.asarray(v, np.float32)[None, :], (128, v.shape[-1]))
    rowp = np.zeros((L_, 128, NR), np.float32)
    colp = np.zeros((L_, 128, NCOL), np.float32)
    bmod2 = np.zeros((L_, 33, 6 * D), np.float32)
    for l in range(L_):
        rowp[l, :, 0:1024] = rep(inp["norm1_g"][l])
        rowp[l, :, 1024:2048] = rep(inp["norm2_g"][l])
        rowp[l, :, 2048:2560] = rep(np.tile(inp["q_norm_g"][l], 8))
        rowp[l, :, 2560:2816] = rep(np.tile(inp["k_norm_g"][l], 4))
        rowp[l, :, 2816:2944] = rep(inp["dn_norm_g"][l])
        rowp[l, :, 2944:2976] = rep(inp["b_router"][l])
        rowp[l, :, 2976:2984] = rep(inp["dn_a_log"][l].reshape(8))
        rowp[l, :, 2984:2992] = rep(inp["dn_dt_bias"][l].reshape(8))
        colp[l, :, 0:48] = inp["dn_conv_w"][l].reshape(4, 12, 128).transpose(2, 1, 0).reshape(128, 48)
        colp[l, :, 48:172] = inp["conf_dw_w"][l].reshape(31, 4, 128).transpose(2, 1, 0).reshape(128, 124)
        colp[l, :, 172:176] = inp["conf_dw_b"][l].reshape(4, 128).T
        colp[l, :, 176:180] = inp["conf_ln_g"][l].reshape(4, 128).T
        colp[l, :, 180:184] = inp["conf_ln_b"][l].reshape(4, 128).T
        colp[l, :, 184:696] = inp["b_gate_up"][l].reshape(NE, 16, 128).transpose(2, 0, 1).reshape(128, 512)
        bmod2[l, 0] = inp["b_mod"][l]
        bmod2[l, 32] = inp["b_mod"][l]
    shared = dict(bmod2=bmod2, rowp=rowp, colp=colp, finalg=np.ascontiguousarray(rep(inp["final_g"])), rope=rope, cmat=cm, cmask=cmask)
    for k in ("w_mod", "w_in", "w_att_o", "w_dn_o", "w_conf_o", "w_out", "w_router", "w_gate_up", "w_down", "b_down"):
        shared[k] = f(inp[k])
    maps = []
    cc = np.asarray(inp["c_ctx"], np.float32).reshape(8, 128).T
    for b in range(8):
        m = dict(shared)
        m["xin"] = np.ascontiguousarray(np.concatenate([inp["ctx"][b], inp["x"][b]], 0).astype(np.float32))
        cv = np.zeros((128, 8, 33), np.float32)
        cv[:, :, 0] = np.asarray(inp["c"][b], np.float32).reshape(8, 128).T
        cv[:, :, 32] = cc
        m["cvec"] = cv
        maps.append(m)
    return maps


_NC = None


def kernel(**inputs):
    global _NC
    maps = _prep(inputs)
    if _NC is None:
        _NC = build()
    res = run_bass_kernel_spmd(_NC, maps, core_ids=list(range(8)))
    return np.stack([np.asarray(r["out"], dtype=np.float32) for r in res.results], 0)
```
